# Optimizing a Trainium2 kernel written in Bass

```python
import math
import jax, jax.numpy as jnp
from jax import lax
import numpy as np

D_MODEL = 1024
BATCH = 16
SEQ = 2048
DEPTH = 1

DIFF_HEADS = 4
DIFF_QK_DIM = 64
DIFF_V_DIM = 2 * DIFF_QK_DIM
DIFF_WIDTH = DIFF_HEADS * DIFF_V_DIM
HGRN_HEADS = 4
HGRN_K_DIM = 128
HGRN_V_DIM = 128
HGRN_WIDTH = HGRN_HEADS * HGRN_V_DIM
HGRN_CHUNK = 32
MIX_WIDTH = DIFF_WIDTH + HGRN_WIDTH
DIFF_Q_COLS = DIFF_HEADS * 2 * DIFF_QK_DIM
HGRN_K_COLS = HGRN_HEADS * HGRN_K_DIM
IN_SPLIT_SIZES = (DIFF_Q_COLS, DIFF_Q_COLS, DIFF_WIDTH, HGRN_K_COLS, HGRN_K_COLS, HGRN_WIDTH, HGRN_WIDTH)
IN_WIDTH = 2 * DIFF_Q_COLS + DIFF_WIDTH + 2 * HGRN_K_COLS + 2 * HGRN_WIDTH
REL_BUCKETS = 32
REL_MAX_DIST = 128
Q_BLOCK = 128
PEER_HEADS = 8
PEER_N_KEYS = 128
PEER_N_EXPERTS = PEER_N_KEYS * PEER_N_KEYS
PEER_QUERY_DIM = 256
PEER_TOPK = 16
PEER_TOKEN_BLOCK = 128
NORM_EPS = 1e-6

kernel_name = 'hymba_diffattn_hgrn2_peer_block'


def rmsnorm(x, g):
    xf = x.astype(jnp.float32)
    y = xf * lax.rsqrt(jnp.mean(xf * xf, axis=-1, keepdims=True) + NORM_EPS)
    return (y * g.astype(jnp.float32)).astype(x.dtype)


def t5_causal_bucket(q_pos, k_pos):
    n = jnp.maximum(q_pos[:, None] - k_pos[None, :], 0)
    max_exact = REL_BUCKETS // 2
    nf = jnp.maximum(n, max_exact).astype(jnp.float32)
    large = max_exact + (jnp.log(nf / max_exact) / math.log(REL_MAX_DIST / max_exact)
                         * (REL_BUCKETS - max_exact)).astype(jnp.int32)
    large = jnp.minimum(large, REL_BUCKETS - 1)
    return jnp.where(n < max_exact, n, large)


def diff_attention(q, k, v, lam, rel_bias_table):
    B, H, _, S, dq = q.shape
    dv = v.shape[-1]
    nb = S // Q_BLOCK
    qb = q.reshape(B, H, 2, nb, Q_BLOCK, dq).transpose(3, 0, 1, 2, 4, 5)
    k_pos = jnp.arange(S)
    scale = DIFF_QK_DIM ** -0.5

    def block(args):
        qblk, bi = args
        q_pos = bi * Q_BLOCK + jnp.arange(Q_BLOCK)
        bucket = t5_causal_bucket(q_pos, k_pos)
        bias = jnp.take(rel_bias_table, bucket, axis=0).transpose(2, 0, 1).astype(jnp.float32)
        logits = jnp.einsum('bhmqd,bhmkd->bhmqk', qblk, k).astype(jnp.float32) * scale + bias[None, :, None]
        causal = k_pos[None, :] <= q_pos[:, None]
        logits = jnp.where(causal, logits, -jnp.inf)
        p = jax.nn.softmax(logits, axis=-1)
        attn = p[:, :, 0] - lam * p[:, :, 1]
        return jnp.einsum('bhqk,bhkd->bhqd', attn.astype(v.dtype), v)

    out = lax.map(block, (qb, jnp.arange(nb)))
    return out.transpose(1, 0, 3, 2, 4).reshape(B, S, H, dv)


def hgrn2_chunked(q, k, v, log_f):
    B, H, S, dk = q.shape
    dv = v.shape[-1]
    C = HGRN_CHUNK
    nc = S // C
    q, k, v, log_f = (t.reshape(B, H, nc, C, t.shape[-1]) for t in (q, k, v, log_f))
    b = jnp.cumsum(log_f, axis=3)
    b_last = b[:, :, :, -1:, :]
    q_dec = q * jnp.exp(b)
    k_inv = k * jnp.exp(-b)
    k_dec = k * jnp.exp(b_last - b)
    causal = jnp.tril(jnp.ones((C, C), dtype=bool))
    scores = jnp.where(causal, jnp.einsum('bhncd,bhnsd->bhncs', q_dec, k_inv), 0.0)
    o_intra = jnp.einsum('bhncs,bhnsv->bhncv', scores, v)
    chunk_decay = jnp.exp(b_last[:, :, :, 0, :])

    def step(state, inp):
        qd, kd, vv, cd = inp
        o = jnp.einsum('bhcd,bhdv->bhcv', qd, state)
        state = cd[..., None] * state + jnp.einsum('bhcd,bhcv->bhdv', kd, vv)
        return state, o

    xs = (jnp.moveaxis(q_dec, 2, 0), jnp.moveaxis(k_dec, 2, 0), jnp.moveaxis(v, 2, 0), jnp.moveaxis(chunk_decay, 2, 0))
    s0 = jnp.zeros((B, H, dk, dv), jnp.float32)
    _, o_inter = lax.scan(step, s0, xs)
    o = o_intra + jnp.moveaxis(o_inter, 0, 2)
    return o.reshape(B, H, S, dv)


def peer(xn, w_q, sub_keys, u, v):
    B, S, D = xn.shape
    T = B * S
    xt = xn.reshape(T, D)
    q = (xt @ w_q).reshape(T, PEER_HEADS, 2, PEER_QUERY_DIM // 2)
    s = jnp.einsum('thcd,hcnd->thcn', q, sub_keys).astype(jnp.float32)
    s1, i1 = lax.top_k(s[:, :, 0], PEER_TOPK)
    s2, i2 = lax.top_k(s[:, :, 1], PEER_TOPK)
    cand = (s1[..., :, None] + s2[..., None, :]).reshape(T, PEER_HEADS, PEER_TOPK * PEER_TOPK)
    cand_idx = (i1[..., :, None] * PEER_N_KEYS + i2[..., None, :]).reshape(T, PEER_HEADS, PEER_TOPK * PEER_TOPK)
    top_s, top_pos = lax.top_k(cand, PEER_TOPK)
    expert_idx = jnp.take_along_axis(cand_idx, top_pos, axis=-1)
    gate = jax.nn.softmax(top_s, axis=-1)
    nblk = T // PEER_TOKEN_BLOCK
    n_sel = PEER_HEADS * PEER_TOPK

    def block(args):
        xb, idx, g = args
        hidden = jnp.einsum('td,tkd->tk', xb, u[idx]).astype(jnp.float32)
        act = jax.nn.gelu(hidden, approximate=False) * g
        return jnp.einsum('tk,tkd->td', act.astype(xb.dtype), v[idx])

    out = lax.map(block, (xt.reshape(nblk, PEER_TOKEN_BLOCK, D),
                          expert_idx.reshape(nblk, PEER_TOKEN_BLOCK, n_sel),
                          gate.reshape(nblk, PEER_TOKEN_BLOCK, n_sel)))
    return out.reshape(B, S, D)


def setup_inputs(seed: int = 0) -> dict:
    key = jax.random.key(seed)
    ks = jax.random.split(key, 18)
    f32 = jnp.float32
    nrm = lambda k, shape, s: jax.random.normal(k, shape, f32) * s
    gain = lambda k, shape: 1.0 + 0.02 * jax.random.normal(k, shape, f32)
    return {
        'x': nrm(ks[0], (BATCH, SEQ, D_MODEL), 1.0),
        'norm1_g': gain(ks[1], (DEPTH, D_MODEL)),
        'w_in': nrm(ks[2], (DEPTH, D_MODEL, IN_WIDTH), D_MODEL ** -0.5),
        'diff_lambda_q1': nrm(ks[3], (DEPTH, DIFF_QK_DIM), 0.1),
        'diff_lambda_k1': nrm(ks[4], (DEPTH, DIFF_QK_DIM), 0.1),
        'diff_lambda_q2': nrm(ks[5], (DEPTH, DIFF_QK_DIM), 0.1),
        'diff_lambda_k2': nrm(ks[6], (DEPTH, DIFF_QK_DIM), 0.1),
        'diff_subln_g': gain(ks[7], (DEPTH, DIFF_V_DIM)),
        'rel_bias_table': nrm(ks[8], (REL_BUCKETS, DIFF_HEADS), 0.5),
        'hgrn_lb_logits': nrm(ks[9], (DEPTH + 1, HGRN_K_COLS), 0.1),
        'hgrn_gnorm_g': gain(ks[10], (DEPTH, HGRN_V_DIM)),
        'w_out': nrm(ks[11], (DEPTH, MIX_WIDTH, D_MODEL), MIX_WIDTH ** -0.5),
        'norm2_g': gain(ks[12], (DEPTH, D_MODEL)),
        'peer_w_q': nrm(ks[13], (DEPTH, D_MODEL, PEER_HEADS * PEER_QUERY_DIM), D_MODEL ** -0.5),
        'peer_sub_keys': nrm(ks[14], (DEPTH, PEER_HEADS, 2, PEER_N_KEYS, PEER_QUERY_DIM // 2), (PEER_QUERY_DIM // 2) ** -0.5),
        'peer_u': nrm(ks[15], (DEPTH, PEER_N_EXPERTS, D_MODEL), D_MODEL ** -0.5),
        'peer_v': nrm(ks[16], (DEPTH, PEER_N_EXPERTS, D_MODEL), PEER_HEADS ** -0.5),
        'final_norm_g': gain(ks[17], (D_MODEL,)),
    }


def reference(x, norm1_g, w_in, diff_lambda_q1, diff_lambda_k1, diff_lambda_q2, diff_lambda_k2,
              diff_subln_g, rel_bias_table, hgrn_lb_logits, hgrn_gnorm_g, w_out, norm2_g,
              peer_w_q, peer_sub_keys, peer_u, peer_v, final_norm_g):
    B, S, _ = x.shape
    split_points = [int(p) for p in np.cumsum(IN_SPLIT_SIZES)[:-1]]
    lb_all = jnp.cumsum(jax.nn.softmax(hgrn_lb_logits.astype(jnp.float32), axis=0), axis=0)
    h = x
    for l in range(DEPTH):
        hn = rmsnorm(h, norm1_g[l])
        proj = hn @ w_in[l]
        dq, dk, dvv, hq, hf, hi, hg = jnp.split(proj, split_points, axis=-1)

        q = dq.reshape(B, S, DIFF_HEADS, 2, DIFF_QK_DIM).transpose(0, 2, 3, 1, 4)
        k = dk.reshape(B, S, DIFF_HEADS, 2, DIFF_QK_DIM).transpose(0, 2, 3, 1, 4)
        v = dvv.reshape(B, S, DIFF_HEADS, DIFF_V_DIM).transpose(0, 2, 1, 3)
        lam_init = 0.8 - 0.6 * math.exp(-0.3 * l)
        lam = (jnp.exp(jnp.sum(diff_lambda_q1[l].astype(jnp.float32) * diff_lambda_k1[l].astype(jnp.float32)))
               - jnp.exp(jnp.sum(diff_lambda_q2[l].astype(jnp.float32) * diff_lambda_k2[l].astype(jnp.float32)))
               + lam_init)
        a = diff_attention(q, k, v, lam, rel_bias_table)
        a = rmsnorm(a, diff_subln_g[l]) * (1.0 - lam_init)

        heads = lambda t: t.reshape(B, S, HGRN_HEADS, -1).transpose(0, 2, 1, 3).astype(jnp.float32)
        lb = lb_all[l].reshape(HGRN_HEADS, 1, HGRN_K_DIM)
        f = lb + (1.0 - lb) * jax.nn.sigmoid(heads(hf))
        o = hgrn2_chunked(jax.nn.silu(heads(hq)), 1.0 - f, heads(hi), jnp.log(f))
        o = o.transpose(0, 2, 1, 3)
        o = rmsnorm(o, hgrn_gnorm_g[l]) * jax.nn.silu(hg.reshape(B, S, HGRN_HEADS, HGRN_V_DIM).astype(jnp.float32))

        mixed = jnp.concatenate([a.reshape(B, S, DIFF_WIDTH).astype(x.dtype),
                                 o.reshape(B, S, HGRN_WIDTH).astype(x.dtype)], axis=-1)
        h = h + mixed @ w_out[l]

        hn2 = rmsnorm(h, norm2_g[l])
        h = h + peer(hn2, peer_w_q[l], peer_sub_keys[l], peer_u[l], peer_v[l])
    return rmsnorm(h, final_norm_g)
```

```python
import math
from contextlib import ExitStack

import numpy as np
import concourse.bass as bass
import concourse.mybir as mybir
from concourse.bass_utils import run_bass_kernel_spmd

F32 = mybir.dt.float32
BF16 = mybir.dt.bfloat16
U32 = mybir.dt.uint32
AF = mybir.ActivationFunctionType
ALU = mybir.AluOpType
AX = mybir.AxisListType
EPS = 1e-6
NCORES = 8
NTOK = 4096
ARENA_WORDS = 52800
SES = True
SAME_ENG_GAP = 0
SKIP = {}
CAND_ENG = "pool"
MUL_ENG = "pool"


class Tok:
    __slots__ = ("name", "w", "r", "parent", "children", "pw", "pr")

    def __init__(self, name, parent=None):
        self.name = name
        self.w = None
        self.r = []
        self.parent = parent
        self.children = []
        self.pw = []
        self.pr = []
        if parent is not None:
            parent.children.append(self)


class Op:
    __slots__ = ("eng", "fn", "deps", "needs_inc", "inc_no", "dma_key", "dma_val", "gen", "pos")


class Prog:
    ENG = ("pe", "act", "dve", "pool", "sp")

    def __init__(self, nc, same_engine_sync=True):
        self.nc = nc
        self.ops = {e: [] for e in self.ENG}
        self.dma_cnt = {}
        self.last_dma = {}
        self.last_cmp = {}
        self.same_engine_sync = same_engine_sync
        self.gen = 0
        self.npos = {e: 0 for e in self.ENG}

    def op(self, eng, fn, reads=(), writes=(), dma=None, extra=()):
        o = Op()
        o.eng, o.fn, o.deps, o.needs_inc, o.inc_no, o.dma_key = eng, fn, [], False, None, dma
        o.dma_val = None
        o.gen = self.gen
        o.pos = self.npos[eng]
        if fn is not None:
            self.npos[eng] += 1
        if dma is not None:
            self.dma_cnt[dma] = self.dma_cnt.get(dma, 0) + 16
            o.dma_val = self.dma_cnt[dma]
        deps = list(extra)
        if dma is not None and dma in self.last_dma:
            deps.append(self.last_dma[dma])
        for t in reads:
            if t.w is not None:
                deps.append(t.w)
            deps.extend(t.pw)
        for t in writes:
            if t.w is not None:
                deps.append(t.w)
            deps.extend(t.r)
            deps.extend(t.pw)
            deps.extend(t.pr)
        seen = set()
        for p in deps:
            if id(p) in seen or p is o:
                continue
            seen.add(id(p))
            if p.dma_key is None:
                if p.eng == eng and (eng == "pe" or not self.same_engine_sync):
                    continue
                if p.eng == eng and eng in ("act", "dve") and SAME_ENG_GAP and o.pos - p.pos > SAME_ENG_GAP:
                    continue
                p.needs_inc = True
            o.deps.append(p)
        def add(lst):
            for i, q in enumerate(lst):
                if q.eng == o.eng and q.dma_key == o.dma_key:
                    lst[i] = o
                    return
            lst.append(o)

        for t in reads:
            add(t.r)
            for c in t.children:
                add(c.r)
            if t.parent is not None:
                add(t.parent.pr)
        for t in writes:
            t.w = o
            t.r = []
            t.pw = []
            t.pr = []
            for c in t.children:
                c.w = o
                c.r = []
            if t.parent is not None:
                add(t.parent.pw)
        self.ops[eng].append(o)
        if fn is not None:
            if dma is not None:
                self.last_dma[dma] = o
            else:
                self.last_cmp[eng] = o
        return o

    def barrier(self):
        deps = list(self.last_cmp.values()) + list(self.last_dma.values())
        for e in self.ENG:
            self.op(e, None, extra=deps)
        self.gen += 1

    def emit(self, final_waits=()):
        nc = self.nc
        ngen = self.gen + 1
        for e in self.ENG:
            c = [0] * ngen
            for o in self.ops[e]:
                if o.needs_inc:
                    c[o.gen] += 1
                    o.inc_no = c[o.gen]
            print("[prog]", e, "ops", len(self.ops[e]), "incs/gen", c)
        with ExitStack() as st:
            esem = {(e, g): st.enter_context(nc.semaphore(f"s_{e}{g}")) for e in self.ENG for g in range(ngen)}
            dsem = {k: st.enter_context(nc.semaphore(f"d_{k}")) for k in self.dma_cnt}
            block = st.enter_context(nc.Block())
            engobj = {"pe": "tensor", "act": "scalar", "dve": "vector", "pool": "gpsimd", "sp": "sync"}

            def make(e):
                def body(eng):
                    waited = {}
                    for o in self.ops[e]:
                        for p in o.deps:
                            if p.dma_key is not None:
                                sem, val, key = dsem[p.dma_key], p.dma_val, ("d", p.dma_key)
                            else:
                                sem, val, key = esem[(p.eng, p.gen)], p.inc_no, ("e", p.eng, p.gen)
                            if waited.get(key, 0) >= val:
                                continue
                            waited[key] = val
                            eng.wait_ge(sem, val)
                        if o.fn is None:
                            continue
                        ins = o.fn(eng)
                        if o.dma_key is not None:
                            ins.then_inc(dsem[o.dma_key], 16)
                        elif o.needs_inc:
                            ins.then_inc(esem[(e, o.gen)], 1)
                    if e == "sp":
                        for p in final_waits:
                            eng.wait_ge(dsem[p.dma_key], p.dma_val)

                return body

            for e in self.ENG:
                getattr(block, engobj[e])(make(e))


class Arena:
    def __init__(self, t, words):
        self.t, self.words, self.off = t, words, 0

    def raw(self, nbytes):
        w = (nbytes + 3) // 4
        assert self.off + w <= self.words, f"arena overflow {self.off + w} > {self.words}"
        ap = self.t[:, self.off:self.off + w]
        self.off += w
        return ap

    def _shape(self, ap, shape):
        if len(shape) == 2:
            return ap
        if len(shape) == 3:
            return ap.rearrange("p (a b) -> p a b", a=shape[1])
        if len(shape) == 4:
            return ap.rearrange("p (a b c) -> p a b c", a=shape[1], b=shape[2])
        raise ValueError

    def f32(self, shape):
        n = int(np.prod(shape[1:]))
        return self._shape(self.raw(n * 4), shape)

    def bf16(self, shape):
        n = int(np.prod(shape[1:]))
        assert n % 2 == 0
        return self._shape(self.raw(n * 2).bitcast(BF16), shape)

    def u32(self, shape):
        n = int(np.prod(shape[1:]))
        return self._shape(self.raw(n * 4).bitcast(U32), shape)


def build(dbg=False, nblk=8, phases=(1, 2, 3), npass=16):
    nc = bass.Bass("TRN2", target_bir_lowering=False)
    dt = nc.dram_tensor
    IN = "ExternalInput"
    x_d = dt("x", [NTOK, 1024], F32, kind=IN).ap()
    win_d = dt("w_in", [1024, 3584], F32, kind=IN).ap()
    wout_d = dt("w_out", [1024, 1024], F32, kind=IN).ap()
    wq_d = dt("w_q", [1024, 2048], F32, kind=IN).ap()
    skT_d = dt("skT", [128, 2048], F32, kind=IN).ap()
    uT_d = dt("uT", [16384, 1024], F32, kind=IN).ap()
    vP_d = dt("vP", [16384, 1024], F32, kind=IN).ap()
    wb_d = dt("wbias", [128, 4, 1024], F32, kind=IN).ap()
    cpk_d = dt("cpk", [128, 848], F32, kind=IN).ap()
    ppk_d = dt("ppk", [128, 540], F32, kind=IN).ap()
    gfin_d = dt("gfin", [128, 1024], F32, kind=IN).ap()
    out_d = dt("out", [NTOK, 1024], F32, kind="ExternalOutput").ap()
    SCR = "ExternalOutput" if dbg else "Internal"
    h_d = dt("h_scr", [NTOK, 1024], F32, kind=SCR).ap()
    hn2T_d = dt("hn2T_scr", [128, 8, NTOK], BF16, kind=SCR).ap()
    rt_d = dt("rt_scr", [128, 3, NTOK], F32, kind=SCR).ap()
    mix_d = dt("mix_scr", [128, 8, NTOK], BF16, kind=SCR).ap() if dbg else None
    ubf_d = dt("ubf", [16384, 1024], BF16, kind="Internal").ap()
    vbf_d = dt("vbf", [16384, 1024], BF16, kind="Internal").ap()

    st = ExitStack()
    arena_t = st.enter_context(nc.sbuf_tensor("arena", [128, ARENA_WORDS], F32))
    psum = st.enter_context(nc.psum_tensor("psum", [128, 8, 512], F32))
    P = Prog(nc, same_engine_sync=SES)
    A = Arena(arena_t, ARENA_WORDS)
    finals = []
    dumps = {}

    def dump(name, ap, toks):
        if not dbg or name in dumps:
            return
        shp = list(ap.shape)
        dd = dt("dbg_" + name, shp, ap.dtype, kind="ExternalOutput").ap()
        dumps[name] = dd
        P.op("sp", lambda e: e.dma_start(out=dd, in_=ap), reads=list(toks), dma="dbg_" + name)

    bank_t = [Tok(f"bank{i}") for i in range(8)]

    def bank(i):
        return psum[:, i, :]

    def bank_bf(i):
        return psum[:, i, :].bitcast(BF16)

    cpk = A.f32([128, 848]); t_cpk = Tok("cpk")
    ppk = A.f32([128, 540]); t_ppk = Tok("ppk")
    iota_f = cpk[:, 0:128]
    ident_f = cpk[:, 128:256]
    maskc = cpk[:, 256:320]
    rmask = cpk[:, 320:832]
    iota16 = cpk[:, 832:848]
    cfar = ppk[:, 0:4]
    lbl = ppk[:, 4:12].rearrange("p (l h) -> p l h", l=2)
    lamv = ppk[:, 12:268].rearrange("p (k d) -> p k d", k=4)
    g1pc = ppk[:, 268:276]
    g2pc = ppk[:, 276:284]
    gsub = ppk[:, 284:412]
    ggn = ppk[:, 412:540]
    ident_bf = A.bf16([128, 128]); t_idb = Tok("identbf")
    sm = A.f32([128, 64]); t_sm = Tok("sm")
    lam_neg = sm[:, 0:1]
    lb = sm[:, 4:8]
    oml = sm[:, 8:12]
    gsub8 = A.f32([128, 128]); t_gsub8 = Tok("gsub8")

    P.op("sp", lambda e: e.dma_start(out=cpk, in_=cpk_d[:, :]), writes=[t_cpk], dma="c0")
    P.op("sp", lambda e: e.dma_start(out=ppk, in_=ppk_d[:, :]), writes=[t_ppk], dma="c1")
    P.op("dve", lambda e: e.tensor_copy(out=ident_bf, in_=ident_f), reads=[t_cpk], writes=[t_idb])
    cv_ops = []

    def issue_conversions():
        for k in range(8):
            r0 = k * 2048
            cv_ops.append(P.op("pool", lambda e, r0=r0: e.dma_start(out=ubf_d[r0:r0 + 2048, :], in_=uT_d[r0:r0 + 2048, :]), dma="cv"))
            cv_ops.append(P.op("pool", lambda e, r0=r0: e.dma_start(out=vbf_d[r0:r0 + 2048, :], in_=vP_d[r0:r0 + 2048, :]), dma="cv"))

    lt = A.f32([128, 2, 64]); t_lt = Tok("lt")
    P.op("dve", lambda e: e.tensor_tensor(out=lt[:, 0, :], in0=lamv[:, 0, :], in1=lamv[:, 1, :], op=ALU.mult), reads=[t_ppk], writes=[t_lt])
    P.op("dve", lambda e: e.tensor_tensor(out=lt[:, 1, :], in0=lamv[:, 2, :], in1=lamv[:, 3, :], op=ALU.mult), reads=[t_ppk, t_lt], writes=[t_lt])
    P.op("dve", lambda e: e.tensor_reduce(out=sm[:, 16:18], in_=lt, axis=AX.X, op=ALU.add), reads=[t_lt], writes=[t_sm])
    P.op("act", lambda e: e.activation(out=sm[:, 18:20], in_=sm[:, 16:18], func=AF.Exp), reads=[t_sm], writes=[t_sm])
    P.op("dve", lambda e: e.tensor_tensor(out=sm[:, 20:21], in0=sm[:, 19:20], in1=sm[:, 18:19], op=ALU.subtract), reads=[t_sm], writes=[t_sm])
    P.op("dve", lambda e: e.tensor_scalar(lam_neg, sm[:, 20:21], -0.2, None, op0=ALU.add), reads=[t_sm], writes=[t_sm])
    P.op("dve", lambda e: e.tensor_tensor(out=sm[:, 24:28], in0=lbl[:, 0, :], in1=lbl[:, 1, :], op=ALU.subtract), reads=[t_ppk, t_sm], writes=[t_sm])
    P.op("act", lambda e: e.activation(out=lb, in_=sm[:, 24:28], func=AF.Sigmoid), reads=[t_sm], writes=[t_sm])
    P.op("dve", lambda e: e.tensor_scalar(oml, lb, -1.0, 1.0, op0=ALU.mult, op1=ALU.add), reads=[t_sm], writes=[t_sm])
    P.op("dve", lambda e: e.tensor_scalar(gsub8, gsub, 0.8, None, op0=ALU.mult), reads=[t_ppk], writes=[t_gsub8])

    persist_mark = A.off

    w_in_bf = A.bf16([128, 8, 3584]); t_win = Tok("win")
    w_out_bf = A.bf16([128, 8, 1024]); t_wout = Tok("wout")
    for c in range(8):
        for hf in range(2):
            P.op("pool", lambda e, c=c, hf=hf: e.dma_start(out=w_in_bf[:, c, hf * 1792:(hf + 1) * 1792],
                                                           in_=win_d[c * 128:(c + 1) * 128, hf * 1792:(hf + 1) * 1792]),
                 writes=[t_win], dma="win")
    for c in range(8):
        P.op("pool", lambda e, c=c: e.dma_start(out=w_out_bf[:, c, :], in_=wout_d[c * 128:(c + 1) * 128, :]), writes=[t_wout], dma="wout")
    issue_conversions()

    wbs = [A.f32([128, 1024]) for _ in range(2)]; t_wbs = [Tok("wb0"), Tok("wb1")]
    kT = A.bf16([128, 4, 2048]); t_kT = Tok("kT"); t_kTb = [Tok(f"kT{j}", t_kT) for j in range(4)]
    Vp = A.bf16([128, 16, 4, 130]); t_Vp = Tok("Vp"); t_Vpb = [Tok(f"Vp{j}", t_Vp) for j in range(16)]
    xnT = A.bf16([128, 8, 512]); t_xnT = Tok("xnT"); t_xnTt = [Tok(f"xnT{i}", t_xnT) for i in range(4)]
    qT = A.bf16([128, 4, 512]); t_qT = [Tok(f"qT{h}") for h in range(4)]
    qdT = A.bf16([128, 4, 512]); t_qdT = [Tok(f"qdT{h}") for h in range(4)]
    kinvT = A.bf16([128, 4, 512]); t_kinvT = [Tok(f"kinvT{h}") for h in range(4)]
    kdec = A.bf16([128, 4, 4, 128]); t_kdec = [Tok(f"kdec{h}") for h in range(4)]
    hv = A.bf16([128, 4, 512]); t_hv = [Tok(f"hv{i}") for i in range(4)]
    hgs = A.bf16([128, 4, 512]); t_hgs = [Tok(f"hgs{i}") for i in range(4)]
    state_f = A.f32([128, 4, 128]); t_stf = [Tok(f"stf{h}") for h in range(4)]
    state_b2 = [A.bf16([128, 4, 128]) for _ in range(2)]; t_stb2 = [[Tok(f"stb{a}{h}") for h in range(4)] for a in range(2)]
    cd = A.f32([128, 4, 8]); t_cd = [Tok(f"cd{h}") for h in range(4)]
    scm = A.bf16([128, 2, 4, 64]); t_scm = [[Tok(f"scm{a}{h}") for h in range(4)] for a in range(2)]
    mixT = A.bf16([128, 8, 512]); t_mixT = Tok("mixT"); t_mixc = [[Tok(f"mix{c}_{t}", t_mixT) for t in range(4)] for c in range(8)]
    hn2Tb = [A.bf16([128, 8, 128]) for _ in range(2)]; t_hn2Tb = [Tok("hn2Tb0"), Tok("hn2Tb1")]
    xh = [A.f32([128, 1024]) for _ in range(2)]; t_xh = [Tok("xh0"), Tok("xh1")]
    NPOOL = 10
    nrm = [A.f32([128, 256]) for _ in range(4)]; t_nrm = [Tok(f"nrm{i}") for i in range(4)]; nrm_i = [0]
    pool_ap = [A.f32([128, 512]) for _ in range(NPOOL)]
    pool_t = [Tok(f"tmp{i}") for i in range(NPOOL)]
    pool_i = [0]
    NSC = 8
    scs = [A.f32([128, 8]) for _ in range(NSC)]; t_scs = [Tok(f"sc{i}") for i in range(NSC)]
    sc_i = [0]

    def tmp():
        i = pool_i[0] % NPOOL
        pool_i[0] += 1
        return pool_ap[i], pool_t[i]

    def newsc():
        i = sc_i[0] % NSC
        sc_i[0] += 1
        return scs[i], t_scs[i]

    mm_i = [0]

    def mmbank():
        i = 1 + (mm_i[0] % 2)
        mm_i[0] += 1
        return i

    tb_i = [0]

    def tbank():
        i = (0, 7)[tb_i[0] % 2]
        tb_i[0] += 1
        return i

    ev_i = [0]

    def evac_eng():
        ev_i[0] += 1
        return ("act", "dve")[ev_i[0] % 2]

    def copy_op(eng, out, in_, reads, writes, scale=None):
        if eng == "act":
            if scale is None:
                P.op("act", lambda e: e.activation(out=out, in_=in_, func=AF.Copy), reads=reads, writes=writes)
            else:
                P.op("act", lambda e: e.activation(out=out, in_=in_, func=AF.Copy, scale=scale), reads=reads, writes=writes)
        else:
            if scale is None:
                P.op("dve", lambda e: e.tensor_copy(out=out, in_=in_), reads=reads, writes=writes)
            else:
                P.op("dve", lambda e: e.tensor_scalar(out, in_, scale, None, op0=ALU.mult), reads=reads, writes=writes)

    def rstd_multi(items):
        scl = [newsc() for _ in items]
        for (src_ap, rd, n, (jv, t_j)), (sc, t_sc) in zip(items, scl):
            P.op("act", lambda e, jv=jv, src_ap=src_ap, sc=sc: e.activation(out=jv, in_=src_ap, func=AF.Square, accum_out=sc[:, 0:1]),
                 reads=list(rd), writes=[t_j, t_sc])
        for (src_ap, rd, n, _), (sc, t_sc) in zip(items, scl):
            P.op("act", lambda e, sc=sc, n=n: e.activation(out=sc[:, 1:2], in_=sc[:, 0:1], func=AF.Ln, scale=1.0 / n, bias=sc[:, 4:5]),
                 reads=[t_sc], writes=[t_sc])
        for (src_ap, rd, n, _), (sc, t_sc) in zip(items, scl):
            P.op("act", lambda e, sc=sc: e.activation(out=sc[:, 2:3], in_=sc[:, 1:2], func=AF.Exp, scale=-0.5), reads=[t_sc], writes=[t_sc])
        return [(sc[:, 2:3], t_sc) for (sc, t_sc) in scl]

    def rstd_from(src_ap, src_reads, n, junk=None):
        sc, t_sc = newsc()
        if junk is None:
            junk_, t_junk = tmp()
            jv = junk_.bitcast(BF16)[:, 0:n]
        else:
            jv, t_junk = junk
        P.op("act", lambda e: e.activation(out=jv, in_=src_ap, func=AF.Square, accum_out=sc[:, 0:1]),
             reads=list(src_reads), writes=[t_junk, t_sc])
        P.op("act", lambda e: e.activation(out=sc[:, 1:2], in_=sc[:, 0:1], func=AF.Ln, scale=1.0 / n, bias=sc[:, 4:5]),
             reads=[t_sc], writes=[t_sc])
        P.op("act", lambda e: e.activation(out=sc[:, 2:3], in_=sc[:, 1:2], func=AF.Exp, scale=-0.5), reads=[t_sc], writes=[t_sc])
        return sc[:, 2:3], t_sc

    for i in range(NSC):
        P.op("dve", lambda e, ap=scs[i][:, 4:5]: e.memset(ap, EPS), writes=[t_scs[i]])
    P.op("dve", lambda e: e.memset(Vp.rearrange("p a b c -> p (a b c)"), 1.0), writes=[t_Vp])

    def phase1_block(s, j):
        blk = s * 4 + j
        for tt in range(4):
            T = blk * 4 + tt
            xs = T % 2
            P.op("sp", lambda e, T=T, xs=xs: e.dma_start(out=xh[xs], in_=x_d[T * 128:(T + 1) * 128, :]), writes=[t_xh[xs]], dma=f"x{xs}")
            rstd, t_r = rstd_from(xh[xs], [t_xh[xs]], 1024)
            xn, t_xn = tmp()
            xnb = xn.bitcast(BF16)
            P.op("dve", lambda e, xnb=xnb, xs=xs, rstd=rstd: e.tensor_scalar(xnb, xh[xs], rstd, None, op0=ALU.mult),
                 reads=[t_xh[xs], t_r], writes=[t_xn])
            tb = tbank()
            for c in range(8):
                P.op("pe", lambda e, tb=tb, c=c, xnb=xnb: e.transpose(out=bank_bf(tb)[:, c * 128:(c + 1) * 128], in_=xnb[:, c * 128:(c + 1) * 128], identity=ident_bf),
                     reads=[t_xn, t_idb], writes=[bank_t[tb]])
            P.op("dve", lambda e, tb=tb, tt=tt: e.tensor_tensor(out=xnT[:, :, tt * 128:(tt + 1) * 128],
                                                               in0=bank_bf(tb).rearrange("p (c t) -> p c t", c=8),
                                                               in1=g1pc.unsqueeze(2).to_broadcast([128, 8, 128]), op=ALU.mult),
                 reads=[bank_t[tb], t_ppk], writes=[t_xnTt[tt]])

        def proj_fm(col0):
            b = mmbank()
            for c in range(8):
                P.op("pe", lambda e, b=b, c=c: e.matmul(bank(b), lhsT=w_in_bf[:, c, col0:col0 + 128], rhs=xnT[:, c, :], start=(c == 0), stop=(c == 7)),
                     reads=[t_win, t_xnT], writes=[bank_t[b]])
            return b

        for h in range(4):
            b = proj_fm(h * 128)
            copy_op("act", qT[:, h, :], bank(b), [bank_t[b]], [t_qT[h]], scale=0.125)
            b = proj_fm(512 + h * 128)
            copy_op("dve", kT[:, h, j * 512:(j + 1) * 512], bank(b), [bank_t[b]], [t_kTb[j]])
        for hp_ in range(2):
            hs_ = (2 * hp_, 2 * hp_ + 1)
            SL = {h: [tmp() for _ in range(5)] for h in hs_}
            for h in hs_:
                (sq, t_sq), (sg, t_sg) = SL[h][0], SL[h][1]
                b = proj_fm(1536 + h * 128)
                P.op("act", lambda e, b=b, sq=sq: e.activation(out=sq, in_=bank(b), func=AF.Silu), reads=[bank_t[b]], writes=[t_sq])
                b = proj_fm(2048 + h * 128)
                P.op("act", lambda e, b=b, sg=sg: e.activation(out=sg, in_=bank(b), func=AF.Sigmoid), reads=[bank_t[b]], writes=[t_sg])
            for h in hs_:
                (sg, t_sg) = SL[h][1]
                P.op("dve", lambda e, sg=sg, h=h: e.tensor_scalar(sg, sg, oml[:, h:h + 1], lb[:, h:h + 1], op0=ALU.mult, op1=ALU.add),
                     reads=[t_sg, t_sm], writes=[t_sg])
            for h in hs_:
                (sg, t_sg), (lf, t_lf) = SL[h][1], SL[h][2]
                P.op("act", lambda e, sg=sg, lf=lf: e.activation(out=lf, in_=sg, func=AF.Ln), reads=[t_sg], writes=[t_lf])
            for h in hs_:
                (lf, t_lf), (bb, t_bb) = SL[h][2], SL[h][3]
                P.op("dve", lambda e, lf=lf, bb=bb: e.tensor_tensor_scan(out=bb, data0=rmask, data1=lf, initial=0.0, op0=ALU.mult, op1=ALU.add),
                     reads=[t_lf, t_cpk], writes=[t_bb])
            for h in hs_:
                (eb, t_eb), (bb, t_bb), (enb, t_enb) = SL[h][2], SL[h][3], SL[h][4]
                P.op("act", lambda e, eb=eb, bb=bb: e.activation(out=eb, in_=bb, func=AF.Exp), reads=[t_bb], writes=[t_eb])
                P.op("act", lambda e, enb=enb, bb=bb: e.activation(out=enb, in_=bb, func=AF.Exp, scale=-1.0), reads=[t_bb], writes=[t_enb])
            for h in hs_:
                (sq, t_sq), (sg, t_sg), (eb, t_eb) = SL[h][0], SL[h][1], SL[h][2]
                P.op("dve", lambda e, sq=sq, eb=eb, h=h: e.tensor_tensor(out=qdT[:, h, :], in0=sq, in1=eb, op=ALU.mult),
                     reads=[t_sq, t_eb], writes=[t_qdT[h]])
                P.op("act", lambda e, eb=eb, h=h: e.activation(out=cd[:, h, :], in_=eb.rearrange("p (c k) -> p c k", k=64)[:, :, 63], func=AF.Copy),
                     reads=[t_eb], writes=[t_cd[h]])
                P.op("dve", lambda e, sg=sg: e.tensor_scalar(sg, sg, -1.0, 1.0, op0=ALU.mult, op1=ALU.add), reads=[t_sg], writes=[t_sg])
            for h in hs_:
                (sg, t_sg), (enb, t_enb) = SL[h][1], SL[h][4]
                P.op("dve", lambda e, sg=sg, enb=enb: e.tensor_tensor(out=enb, in0=sg, in1=enb, op=ALU.mult), reads=[t_sg, t_enb], writes=[t_enb])
            for h in hs_:
                (kd, t_kd), (enb, t_enb) = SL[h][3], SL[h][4]
                P.op("act", lambda e, enb=enb, h=h: e.activation(out=kinvT[:, h, :], in_=enb, func=AF.Copy), reads=[t_enb], writes=[t_kinvT[h]])
                kdb = kd.bitcast(BF16)[:, 0:512]
                P.op("dve", lambda e, enb=enb, kdb=kdb, h=h: e.tensor_tensor(out=kdb.rearrange("p (c k) -> p c k", k=64),
                                                                           in0=enb.rearrange("p (c k) -> p c k", k=64),
                                                                           in1=cd[:, h, :].unsqueeze(2).to_broadcast([128, 8, 64]), op=ALU.mult),
                     reads=[t_enb, t_cd[h]], writes=[t_kd])
            for h in hs_:
                (kd, t_kd) = SL[h][3]
                kdb = kd.bitcast(BF16)[:, 0:512]
                tb = tbank()
                for tt in range(4):
                    P.op("pe", lambda e, tb=tb, tt=tt, kdb=kdb: e.transpose(out=bank_bf(tb)[:, tt * 128:(tt + 1) * 128], in_=kdb[:, tt * 128:(tt + 1) * 128], identity=ident_bf),
                         reads=[t_kd, t_idb], writes=[bank_t[tb]])
                copy_op("act", kdec[:, :, h, :], bank_bf(tb)[:, 0:512].rearrange("p (t k) -> p t k", t=4), [bank_t[tb]], [t_kdec[h]])

        for tt in range(4):
            for gi, col0 in enumerate((1024, 2560, 3072)):
                b = mmbank()
                for c in range(8):
                    P.op("pe", lambda e, b=b, c=c, tt=tt, col0=col0: e.matmul(bank(b), lhsT=xnT[:, c, tt * 128:(tt + 1) * 128], rhs=w_in_bf[:, c, col0:col0 + 512], start=(c == 0), stop=(c == 7)),
                         reads=[t_win, t_xnTt[tt]], writes=[bank_t[b]])
                if gi == 0:
                    kt = j * 4 + tt
                    copy_op("act", Vp[:, kt, :, 0:128], bank(b).rearrange("p (h d) -> p h d", h=4), [bank_t[b]], [t_Vpb[kt]])
                elif gi == 1:
                    copy_op("dve", hv[:, tt, :], bank(b), [bank_t[b]], [t_hv[tt]])
                else:
                    P.op("act", lambda e, b=b, tt=tt: e.activation(out=hgs[:, tt, :], in_=bank(b), func=AF.Silu), reads=[bank_t[b]], writes=[t_hgs[tt]])

        if blk == 0:
            dump("xnT", xnT, [t_xnT])
            dump("qT", qT, t_qT)
            dump("kT", kT[:, :, 0:512], [t_kT])
            dump("Vp", Vp[:, 0:4, :, :], [t_Vp])
            dump("qdT", qdT, t_qdT)
            dump("kinvT", kinvT, t_kinvT)
            dump("kdec", kdec, t_kdec)
            dump("hv", hv, t_hv)
            dump("hgs", hgs, t_hgs)
            dump("cd", cd, t_cd)
            dump("sm", sm, [t_sm])
            dump("scs0", scs[0], [t_scs[0]])
        nkt = 4 * j + 4
        SB = (1, 2, 0, 7)
        s_i = [0]
        first = {}

        def emit_S(h, kt, m):
            ws = h % 2
            if kt == 0 and m == 0:
                P.op("sp", lambda e: e.dma_start(out=wbs[ws], in_=wb_d[:, h, :]), writes=[t_wbs[ws]], dma=f"wb{ws}")
            qs_min = max(0, kt - 4 * j)
            c0 = qs_min * 128
            sb_ = SB[s_i[0] % 4]
            s_i[0] += 1
            P.op("pe", lambda e: e.matmul(bank(sb_)[:, c0:512], lhsT=kT[m * 64:(m + 1) * 64, h, kt * 128:(kt + 1) * 128],
                                          rhs=qT[m * 64:(m + 1) * 64, h, c0:512], start=True, stop=True),
                 reads=[t_kTb[kt // 4], t_qT[h]], writes=[bank_t[sb_]])
            E, t_E = tmp()
            Eb = E.bitcast(BF16)
            if kt <= 4 * j - 2:
                P.op("act", lambda e: e.activation(out=Eb[:, 0:512], in_=bank(sb_), func=AF.Exp, bias=cfar[:, h:h + 1]),
                     reads=[bank_t[sb_], t_ppk], writes=[t_E])
            else:
                delta = (4 * j - kt) * 128
                w0 = delta + 384 + c0
                ts_, t_ts = tmp()
                P.op("dve", lambda e: e.tensor_tensor(out=ts_[:, c0:512], in0=bank(sb_)[:, c0:512], in1=wbs[ws][:, w0:w0 + 512 - c0], op=ALU.add),
                     reads=[bank_t[sb_], t_wbs[ws]], writes=[t_ts])
                P.op("act", lambda e: e.activation(out=Eb[:, c0:512], in_=ts_[:, c0:512], func=AF.Exp),
                     reads=[t_ts], writes=[t_E])
            return Eb, t_E, qs_min

        def emit_PV(h, kt, m, Eb, t_E, qs_min):
            for qs in range(qs_min, 4):
                ob = 3 + m * 2 + qs // 2
                last = (kt == 4 * j + qs)
                st_ = (h, ob) not in first
                first[(h, ob)] = True
                oap = bank(ob)[:, 0:258].rearrange("p (a d) -> p a d", a=2)[:, qs % 2, :]
                P.op("pe", lambda e, oap=oap, qs=qs, st_=st_, last=last: e.matmul(oap, lhsT=Eb[:, qs * 128:(qs + 1) * 128], rhs=Vp[:, kt, h, 0:129], start=st_, stop=last, skip_group_check=True),
                     reads=[t_E, t_Vpb[kt]], writes=[bank_t[ob]])

        def emit_norm(h):
            Os = []
            for qs in range(4):
                O0 = bank(3 + qs // 2)[:, 0:258].rearrange("p (a d) -> p a d", a=2)[:, qs % 2, :]
                O1 = bank(5 + qs // 2)[:, 0:258].rearrange("p (a d) -> p a d", a=2)[:, qs % 2, :]
                ni = nrm_i[0] % 4
                nrm_i[0] += 1
                sc, t_sc = newsc()
                Os.append((O0, O1, bank_t[3 + qs // 2], bank_t[5 + qs // 2], nrm[ni], t_nrm[ni], sc, t_sc))
            for (O0, O1, tO0, tO1, a_, t_a, sc, t_sc) in Os:
                P.op("dve", lambda e, sc=sc, O0=O0: e.reciprocal(out=sc[:, 5:6], in_=O0[:, 128:129]), reads=[tO0], writes=[t_sc])
            for (O0, O1, tO0, tO1, a_, t_a, sc, t_sc) in Os:
                P.op("dve", lambda e, sc=sc, O1=O1: e.reciprocal(out=sc[:, 6:7], in_=O1[:, 128:129]), reads=[tO1, t_sc], writes=[t_sc])
            for (O0, O1, tO0, tO1, a_, t_a, sc, t_sc) in Os:
                P.op("dve", lambda e, sc=sc: e.tensor_tensor(out=sc[:, 7:8], in0=sc[:, 6:7], in1=lam_neg, op=ALU.mult), reads=[t_sc, t_sm], writes=[t_sc])
            for (O0, O1, tO0, tO1, a_, t_a, sc, t_sc) in Os:
                av = a_[:, 0:128]
                P.op("dve", lambda e, av=av, O0=O0, sc=sc: e.tensor_scalar(av, O0[:, 0:128], sc[:, 5:6], None, op0=ALU.mult), reads=[tO0, t_sc], writes=[t_a])
            for (O0, O1, tO0, tO1, a_, t_a, sc, t_sc) in Os:
                av = a_[:, 0:128]
                P.op("dve", lambda e, av=av, O1=O1, sc=sc: e.scalar_tensor_tensor(out=av, in0=O1[:, 0:128], scalar=sc[:, 7:8], in1=av, op0=ALU.mult, op1=ALU.add),
                     reads=[tO1, t_sc, t_a], writes=[t_a])
            rs = rstd_multi([(a_[:, 0:128], [t_a], 128, (a_.bitcast(BF16)[:, 384:512], t_a)) for (_, _, _, _, a_, t_a, _, _) in Os])
            tb = tbank()
            for qs, ((O0, O1, tO0, tO1, a_, t_a, sc, t_sc), (rstd, t_r)) in enumerate(zip(Os, rs)):
                av = a_[:, 0:128]
                an = a_.bitcast(BF16)[:, 256:384]
                P.op("dve", lambda e, an=an, av=av, rstd=rstd: e.scalar_tensor_tensor(out=an, in0=av, scalar=rstd, in1=gsub8, op0=ALU.mult, op1=ALU.mult),
                     reads=[t_a, t_r, t_gsub8], writes=[t_a])
            for qs, (O0, O1, tO0, tO1, a_, t_a, sc, t_sc) in enumerate(Os):
                an = a_.bitcast(BF16)[:, 256:384]
                P.op("pe", lambda e, qs=qs, an=an: e.transpose(out=bank_bf(tb)[:, qs * 128:(qs + 1) * 128], in_=an, identity=ident_bf), reads=[t_a, t_idb], writes=[bank_t[tb]])
            copy_op("act", mixT[:, h, :], bank_bf(tb)[:, 0:512], [bank_t[tb]], [t_mixc[h][qs] for qs in range(4)])

        LOOK = 3
        tiles = [] if SKIP.get('b1') else [(h, kt, m) for h in range(4) for kt in range(nkt) for m in range(2)]
        pend = []
        nxt = 0
        for i_, (h, kt, m) in enumerate(tiles):
            while nxt < len(tiles) and nxt <= i_ + LOOK:
                pend.append(emit_S(*tiles[nxt]))
                nxt += 1
            emit_PV(h, kt, m, *pend.pop(0))
            if kt == nkt - 1 and m == 1:
                emit_norm(h)

        BD = (4, 5, 6, 1)
        for tt in (range(4) if not SKIP.get('b2') else ()):
            for half in range(2):
                c = tt * 2 + half
                ps_ = slice(half * 64, half * 64 + 64)
                for h in range(4):
                    scT = bank(BD[h])[ps_, 0:64]
                    P.op("pe", lambda e, scT=scT, h=h, c=c: e.matmul(scT, lhsT=kinvT[:, h, c * 64:(c + 1) * 64], rhs=qdT[:, h, c * 64:(c + 1) * 64], start=True, stop=True),
                         reads=[t_kinvT[h], t_qdT[h]], writes=[bank_t[BD[h]]])
                    P.op("dve", lambda e, scT=scT, h=h, half=half, ps_=ps_: e.tensor_tensor(out=scm[ps_, half, h, :], in0=scT, in1=maskc[ps_, :], op=ALU.mult),
                         reads=[bank_t[BD[h]], t_cpk], writes=[t_scm[half][h]])
            for half in range(2):
                c = tt * 2 + half
                par = c % 2
                ps_ = slice(half * 64, half * 64 + 64)
                for h in range(4):
                    oap = bank(3)[ps_, h * 128:(h + 1) * 128]
                    P.op("pe", lambda e, oap=oap, h=h, half=half, ps_=ps_, tt=tt: e.matmul(oap, lhsT=scm[ps_, half, h, :], rhs=hv[ps_, tt, h * 128:(h + 1) * 128], start=(h == 0), stop=False, skip_group_check=True),
                         reads=[t_scm[half][h], t_hv[tt]], writes=[bank_t[3]])
                    dsap = bank(BD[h])[:, 128:256]
                    P.op("pe", lambda e, dsap=dsap, h=h, ps_=ps_, tt=tt: e.matmul(dsap, lhsT=kdec[ps_, tt, h, :], rhs=hv[ps_, tt, h * 128:(h + 1) * 128], start=True, stop=True),
                         reads=[t_kdec[h], t_hv[tt]], writes=[bank_t[BD[h]]])
                    P.op("dve", lambda e, dsap=dsap, h=h, c=c: e.scalar_tensor_tensor(out=state_f[:, h, :], in0=state_f[:, h, :], scalar=cd[:, h, c:c + 1], in1=dsap, op0=ALU.mult, op1=ALU.add),
                         reads=[bank_t[BD[h]], t_cd[h], t_stf[h]], writes=[t_stf[h]])
                    P.op("act", lambda e, h=h, par=par: e.activation(out=state_b2[1 - par][:, h, :], in_=state_f[:, h, :], func=AF.Copy), reads=[t_stf[h]], writes=[t_stb2[1 - par][h]])
                for h in range(4):
                    oap = bank(3)[ps_, h * 128:(h + 1) * 128]
                    P.op("pe", lambda e, oap=oap, h=h, c=c, par=par: e.matmul(oap, lhsT=qdT[:, h, c * 64:(c + 1) * 64], rhs=state_b2[par][:, h, :], start=False, stop=(h == 3), skip_group_check=True),
                         reads=[t_qdT[h], t_stb2[par][h]], writes=[bank_t[3]])
            ot, t_ot = tmp()
            P.op("act", lambda e, ot=ot: e.activation(out=ot, in_=bank(3), func=AF.Copy), reads=[bank_t[3]], writes=[t_ot])
            jk, t_jk = tmp()
            rs = rstd_multi([(ot[:, h * 128:(h + 1) * 128], [t_ot], 128, (jk.bitcast(BF16)[:, h * 128:(h + 1) * 128], t_jk)) for h in range(4)])
            onb = jk.bitcast(BF16)[:, 512:1024]
            for h in range(4):
                ov = ot[:, h * 128:(h + 1) * 128]
                rstd, t_r = rs[h]
                P.op("dve", lambda e, ov=ov, rstd=rstd: e.scalar_tensor_tensor(out=ov, in0=ov, scalar=rstd, in1=ggn, op0=ALU.mult, op1=ALU.mult),
                     reads=[t_ot, t_r, t_ppk], writes=[t_ot])
            P.op("dve", lambda e, ot=ot, onb=onb, tt=tt: e.tensor_tensor(out=onb, in0=ot, in1=hgs[:, tt, :], op=ALU.mult),
                 reads=[t_ot, t_hgs[tt], t_jk], writes=[t_jk])
            tb = tbank()
            for h in range(4):
                P.op("pe", lambda e, tb=tb, h=h, onb=onb: e.transpose(out=bank_bf(tb)[:, h * 128:(h + 1) * 128], in_=onb[:, h * 128:(h + 1) * 128], identity=ident_bf),
                     reads=[t_jk, t_idb], writes=[bank_t[tb]])
            copy_op("act", mixT[:, 4:8, tt * 128:(tt + 1) * 128], bank_bf(tb)[:, 0:512].rearrange("p (h k) -> p h k", h=4), [bank_t[tb]], [t_mixc[4 + h][tt] for h in range(4)])

        if dbg:
            P.op("sp", lambda e, blk=blk: e.dma_start(out=mix_d[:, :, blk * 512:(blk + 1) * 512], in_=mixT), reads=[t_mixT], dma="dbgmix")
        def reload_x(T):
            xs = T % 2
            P.op("sp", lambda e: e.dma_start(out=xh[xs], in_=x_d[T * 128:(T + 1) * 128, :]), writes=[t_xh[xs]], dma=f"x{xs}")

        reload_x(blk * 4)
        for tt in range(4):
            T = blk * 4 + tt
            xs = T % 2
            if tt + 1 < 4:
                reload_x(T + 1)
            for dh in range(2):
                for c in range(8):
                    P.op("pe", lambda e, dh=dh, c=c, tt=tt: e.matmul(bank(1 + dh), lhsT=mixT[:, c, tt * 128:(tt + 1) * 128], rhs=w_out_bf[:, c, dh * 512:(dh + 1) * 512], start=(c == 0), stop=(c == 7)),
                         reads=[t_wout, t_mixc[c][tt]], writes=[bank_t[1 + dh]])
            P.op("dve", lambda e, xs=xs: e.tensor_tensor(out=xh[xs].rearrange("p (a b) -> p a b", a=2), in0=xh[xs].rearrange("p (a b) -> p a b", a=2), in1=psum[:, 1:3, :], op=ALU.add),
                 reads=[bank_t[1], bank_t[2], t_xh[xs]], writes=[t_xh[xs]])
            P.op("sp", lambda e, T=T, xs=xs: e.dma_start(out=h_d[T * 128:(T + 1) * 128, :], in_=xh[xs]), reads=[t_xh[xs]], dma=f"hs{xs}")
            rstd, t_r = rstd_from(xh[xs], [t_xh[xs]], 1024)
            hn, t_hn = tmp()
            hnb = hn.bitcast(BF16)
            P.op("dve", lambda e, hnb=hnb, xs=xs, rstd=rstd: e.tensor_scalar(hnb, xh[xs], rstd, None, op0=ALU.mult), reads=[t_xh[xs], t_r], writes=[t_hn])
            tb = tbank()
            for c in range(8):
                P.op("pe", lambda e, tb=tb, c=c, hnb=hnb: e.transpose(out=bank_bf(tb)[:, c * 128:(c + 1) * 128], in_=hnb[:, c * 128:(c + 1) * 128], identity=ident_bf),
                     reads=[t_hn, t_idb], writes=[bank_t[tb]])
            P.op("dve", lambda e, tb=tb, xs=xs: e.tensor_tensor(out=hn2Tb[xs], in0=bank_bf(tb).rearrange("p (c t) -> p c t", c=8),
                                                               in1=g2pc.unsqueeze(2).to_broadcast([128, 8, 128]), op=ALU.mult),
                 reads=[bank_t[tb], t_ppk], writes=[t_hn2Tb[xs]])
            P.op("sp", lambda e, T=T, xs=xs: e.dma_start(out=hn2T_d[:, :, T * 128:(T + 1) * 128], in_=hn2Tb[xs]), reads=[t_hn2Tb[xs]], dma=f"hn{xs}")

    for s in range(2):
        for h in range(4):
            P.op("dve", lambda e, h=h: e.memset(state_f[:, h, :], 0.0), writes=[t_stf[h]])
            P.op("dve", lambda e, h=h: e.memset(state_b2[0][:, h, :], 0.0), writes=[t_stb2[0][h]])
            P.op("dve", lambda e, h=h: e.memset(state_b2[1][:, h, :], 0.0), writes=[t_stb2[1][h]])
        for j in range(4):
            if s * 4 + j < nblk:
                phase1_block(s, j)

    P.barrier()
    if 2 not in phases:
        P.emit(final_waits=list(P.last_dma.values()))
        st.close()
        return nc
    A.off = persist_mark
    wq_bf = A.bf16([128, 8, 2048]); t_wq = Tok("wq")
    skT_bf = A.bf16([128, 16, 128]); t_sk = Tok("sk")
    for c in range(8):
        P.op("pool", lambda e, c=c: e.dma_start(out=wq_bf[:, c, :], in_=wq_d[c * 128:(c + 1) * 128, :]), writes=[t_wq], dma="wq")
    P.op("pool", lambda e: e.dma_start(out=skT_bf.rearrange("p a b -> p (a b)"), in_=skT_d[:, :]), writes=[t_sk], dma="sk")
    hnb_ = [A.bf16([128, 8, 512]) for _ in range(2)]; t_hnb = [Tok("hnb0"), Tok("hnb1")]
    qpT2 = [A.bf16([128, 16, 512]) for _ in range(2)]; t_qp2 = [[Tok(f"qp{a}_{i}") for i in range(16)] for a in range(2)]
    s_sb2 = [A.f32([128, 16, 128]) for _ in range(2)]; t_s2 = [[Tok(f"s{a}_{i}") for i in range(16)] for a in range(2)]
    wk16 = [A.f32([128, 128]) for _ in range(16)]; t_wk16 = [Tok(f"wk16_{i}") for i in range(16)]
    wk8 = [A.f32([128, 256]) for _ in range(8)]; t_wk8 = [Tok(f"wk8_{i}") for i in range(8)]
    vals = A.f32([128, 16, 16]); t_vals = Tok("vals"); t_valsc = [Tok(f"vals{i}", t_vals) for i in range(16)]
    idx = A.u32([128, 16, 16]); t_idx = Tok("idx"); t_idxc = [Tok(f"idx{i}", t_idx) for i in range(16)]
    idxf = A.f32([128, 16, 16]); t_idxf = Tok("idxf")
    cand = A.f32([128, 8, 256]); t_cand = Tok("cand")
    tv = A.f32([128, 8, 16]); t_tv = Tok("tv"); t_tvh = [Tok(f"tv{i}", t_tv) for i in range(8)]
    pos = A.u32([128, 8, 16]); t_pos = Tok("pos"); t_posh = [Tok(f"pos{i}", t_pos) for i in range(8)]
    pab = A.u32([128, 2, 128]); t_pab = [Tok("pab0"), Tok("pab1")]
    pabf = A.f32([128, 2, 128]); t_pabf = Tok("pabf")
    big = [A.f32([128, 2048]) for _ in range(2)]; t_big = [Tok("big0"), Tok("big1")]
    res3 = A.f32([128, 3, 128]); t_res3 = [Tok("res3_0"), Tok("res3_1"), Tok("res3_2")]
    zs = A.f32([128, 16]); t_zs = Tok("zs")
    rtT = [A.f32([128, 3, 128]) for _ in range(2)]; t_rtT = [Tok("rtT0"), Tok("rtT1")]
    NEG = -1.0e30

    def top16_staged(groups):
        for (src, n, vout, iout, rs_, t_v, t_i, w_, t_w) in groups:
            P.op("dve", lambda e, src=src, vout=vout: e.max(out=vout[:, 0:8], in_=src), reads=rs_, writes=[t_v])
        for (src, n, vout, iout, rs_, t_v, t_i, w_, t_w) in groups:
            P.op("dve", lambda e, src=src, vout=vout, iout=iout: e.max_index(out=iout[:, 0:8], in_max=vout[:, 0:8], in_values=src), reads=rs_ + [t_v], writes=[t_i])
        for (src, n, vout, iout, rs_, t_v, t_i, w_, t_w) in groups:
            P.op("dve", lambda e, src=src, vout=vout, w_=w_, n=n: e.match_replace(out=w_[:, 0:n], in_to_replace=vout[:, 0:8], in_values=src, imm_value=NEG), reads=rs_ + [t_v], writes=[t_w])
        for (src, n, vout, iout, rs_, t_v, t_i, w_, t_w) in groups:
            P.op("dve", lambda e, vout=vout, w_=w_, n=n: e.max(out=vout[:, 8:16], in_=w_[:, 0:n]), reads=[t_w], writes=[t_v])
        for (src, n, vout, iout, rs_, t_v, t_i, w_, t_w) in groups:
            P.op("dve", lambda e, vout=vout, iout=iout, w_=w_, n=n: e.max_index(out=iout[:, 8:16], in_max=vout[:, 8:16], in_values=w_[:, 0:n]), reads=[t_w, t_v], writes=[t_i])

    v4 = vals.rearrange("p (h c) k -> p h c k", c=2)
    i4 = idxf.rearrange("p (h c) k -> p h c k", c=2)
    gsel = res3[:, 2, :].rearrange("p (h k) -> p h k", h=8)
    posf = pos.rearrange("p h k -> p (h k)")
    pabf4 = pabf.rearrange("p a (h k) -> p a h k", h=8)

    def block_prologue(blk, hcs=range(16)):
        hs = blk % 2
        qpT, t_qp = qpT2[hs], t_qp2[hs]
        if 0 in hcs:
            P.op("sp", lambda e: e.dma_start(out=hnb_[hs], in_=hn2T_d[:, :, blk * 512:(blk + 1) * 512]), writes=[t_hnb[hs]], dma=f"hb{hs}")
        for hc in hcs:
            b = mmbank()
            for c in range(8):
                P.op("pe", lambda e, b=b, c=c, hc=hc: e.matmul(bank(b), lhsT=wq_bf[:, c, hc * 128:(hc + 1) * 128], rhs=hnb_[hs][:, c, :], start=(c == 0), stop=(c == 7)),
                     reads=[t_wq, t_hnb[hs]], writes=[bank_t[b]])
            copy_op("act", qpT[:, hc, :], bank(b), [bank_t[b]], [t_qp[hc]])

    def stageA(T):
        blk, tt = T // 4, T % 4
        if T == 0:
            block_prologue(0)
        qpT, t_qp = qpT2[blk % 2], t_qp2[blk % 2]
        s_sb, t_s = s_sb2[T % 2], t_s2[T % 2]
        for hc in range(16):
            sb_ = 3 + hc // 4
            P.op("pe", lambda e, sb_=sb_, hc=hc: e.matmul(bank(sb_)[:, (hc % 4) * 128:(hc % 4 + 1) * 128], lhsT=qpT[:, hc, tt * 128:(tt + 1) * 128], rhs=skT_bf[:, hc, :], start=True, stop=True),
                 reads=[t_qp[hc], t_sk], writes=[bank_t[sb_]])
        for g in range(4):
            copy_op("act", s_sb[:, g * 4:(g + 1) * 4, :], bank(3 + g).rearrange("p (a b) -> p a b", a=4), [bank_t[3 + g]], [t_s[g * 4 + i] for i in range(4)])
        if blk + 1 < 8:
            block_prologue(blk + 1, range(tt * 4, tt * 4 + 4))
        top16_staged([(s_sb[:, hc, :], 128, vals[:, hc, :], idx[:, hc, :], [t_s[hc]], t_valsc[hc], t_idxc[hc], wk16[hc], t_wk16[hc]) for hc in range(16)])
        P.op("dve", lambda e: e.tensor_copy(out=idxf, in_=idx), reads=[t_idx], writes=[t_idxf])
        P.op(CAND_ENG, lambda e: e.tensor_tensor(out=cand.rearrange("p h (a b) -> p h a b", a=16),
                                               in0=v4[:, :, 0, :].unsqueeze(3).to_broadcast([128, 8, 16, 16]),
                                               in1=v4[:, :, 1, :].unsqueeze(2).to_broadcast([128, 8, 16, 16]), op=ALU.add),
             reads=[t_vals], writes=[t_cand])

    def stageB(T):
        top16_staged([(cand[:, h, :], 256, tv[:, h, :], pos[:, h, :], [t_cand], t_tvh[h], t_posh[h], wk8[h], t_wk8[h]) for h in range(8)])
        P.op("dve", lambda e: e.tensor_single_scalar(out=pab[:, 0, :], in_=posf, scalar=4, op=ALU.logical_shift_right), reads=[t_pos], writes=[t_pab[0]])
        P.op("dve", lambda e: e.tensor_single_scalar(out=pab[:, 1, :], in_=posf, scalar=15, op=ALU.bitwise_and), reads=[t_pos], writes=[t_pab[1]])
        P.op("dve", lambda e: e.tensor_tensor(out=gsel, in0=tv, in1=tv[:, :, 0:1].to_broadcast([128, 8, 16]), op=ALU.subtract), reads=[t_tv], writes=[t_res3[2]])
        P.op("act", lambda e: e.activation(out=gsel, in_=gsel, func=AF.Exp), reads=[t_res3[2]], writes=[t_res3[2]])
        P.op("dve", lambda e: e.tensor_copy(out=pabf, in_=pab), reads=t_pab, writes=[t_pabf])
        for ab in range(2):
            bg4 = big[ab].rearrange("p (h k a) -> p h k a", h=8, k=16)
            P.op("dve", lambda e, ab=ab, bg4=bg4: e.tensor_tensor(out=bg4, in0=pabf4[:, ab, :, :].unsqueeze(3).to_broadcast([128, 8, 16, 16]),
                                                              in1=iota16.unsqueeze(1).unsqueeze(1).to_broadcast([128, 8, 16, 16]), op=ALU.is_equal),
                 reads=[t_pabf, t_cpk], writes=[t_big[ab]])
            P.op(MUL_ENG, lambda e, ab=ab, bg4=bg4: e.tensor_tensor(out=bg4, in0=bg4, in1=i4[:, :, ab, :].unsqueeze(2).to_broadcast([128, 8, 16, 16]), op=ALU.mult),
                 reads=[t_idxf, t_big[ab]], writes=[t_big[ab]])
        P.op("dve", lambda e: e.tensor_reduce(out=zs[:, 0:8], in_=gsel, axis=AX.X, op=ALU.add), reads=[t_res3[2]], writes=[t_zs])
        P.op("dve", lambda e: e.reciprocal(out=zs[:, 8:16], in_=zs[:, 0:8]), reads=[t_zs], writes=[t_zs])
        P.op("dve", lambda e: e.tensor_tensor(out=gsel, in0=gsel, in1=zs[:, 8:16].unsqueeze(2).to_broadcast([128, 8, 16]), op=ALU.mult), reads=[t_res3[2], t_zs], writes=[t_res3[2]])

    def stageC(T):
        for ab in range(2):
            P.op("dve", lambda e, ab=ab: e.tensor_reduce(out=res3[:, ab, :], in_=big[ab].rearrange("p (q a) -> p q a", a=16), axis=AX.X, op=ALU.add),
                 reads=[t_big[ab]], writes=[t_res3[ab]])
        tb = tbank()
        for k in range(3):
            P.op("pe", lambda e, tb=tb, k=k: e.transpose(out=bank(tb)[:, k * 128:(k + 1) * 128], in_=res3[:, k, :], identity=ident_f),
                 reads=[t_res3[k], t_cpk], writes=[bank_t[tb]])
        rs = T % 2
        copy_op("act", rtT[rs], bank(tb)[:, 0:384].rearrange("p (k t) -> p k t", k=3), [bank_t[tb]], [t_rtT[rs]])
        P.op("sp", lambda e: e.dma_start(out=rt_d[:, :, T * 128:(T + 1) * 128], in_=rtT[rs]), reads=[t_rtT[rs]], dma=f"rt{rs}")

    NT2 = 32
    stageA(0)
    for T in range(NT2):
        stageB(T)
        if T + 1 < NT2:
            stageA(T + 1)
        stageC(T)

    P.barrier()
    if 3 not in phases:
        P.emit(final_waits=list(P.last_dma.values()))
        st.close()
        return nc
    A.off = persist_mark
    TP = 256
    NPASS = NTOK // TP
    gfin = A.f32([128, 1024]); t_gfin = Tok("gfin")
    P.op("sp", lambda e: e.dma_start(out=gfin, in_=gfin_d[:, :]), writes=[t_gfin], dma="c3")
    NG = 2
    G = [A.bf16([128, TP, 128]) for _ in range(NG)]
    t_G = [Tok(f"G{i}") for i in range(NG)]
    t_Gq = [[Tok(f"G{i}_{q}", t_G[i]) for q in range(TP // 4)] for i in range(NG)]
    NUV = 4
    ub = [A.bf16([128, 8, 128]) for _ in range(NUV)]; t_ub = [Tok(f"ub{i}") for i in range(NUV)]
    vb = [A.bf16([128, 1024]) for _ in range(NUV)]; t_vb = [Tok(f"vb{i}") for i in range(NUV)]
    hp = [A.bf16([128, 8, TP]) for _ in range(2)]; t_hp = [Tok("hp0"), Tok("hp1")]
    rt = [A.f32([128, 3, TP]) for _ in range(2)]; t_rt = [Tok("rt0"), Tok("rt1")]
    NPQ = 8
    Pb = [A.bf16([128, 128]) for _ in range(NPQ)]; t_Pb = [Tok(f"Pb{i}") for i in range(NPQ)]
    Qb = [A.bf16([128, 128]) for _ in range(NPQ)]; t_Qb = [Tok(f"Qb{i}") for i in range(NPQ)]
    NGE = 4
    geb = [A.bf16([128, TP]) for _ in range(NGE)]; t_geb = [Tok(f"ge{i}") for i in range(NGE)]
    agb = [A.bf16([128, TP]) for _ in range(NGE)]; t_agb = [Tok(f"ag{i}") for i in range(NGE)]
    ht = [A.f32([128, 1024]) for _ in range(2)]; t_ht = [Tok("ht0"), Tok("ht1")]
    NP3 = 2
    pool_ap[:] = [A.f32([128, 512]) for _ in range(NP3)] * (NPOOL // NP3)
    pool_t[:] = [Tok(f"tmp3_{i}") for i in range(NP3)] * (NPOOL // NP3)
    scs[:] = [A.f32([128, 8]) for _ in range(NSC)]
    t_scs[:] = [Tok(f"sc3_{i}") for i in range(NSC)]
    for i in range(NSC):
        P.op("dve", lambda e, ap=scs[i][:, 4:5]: e.memset(ap, EPS), writes=[t_scs[i]])
    cv_deps = list(cv_ops)
    npass_run = min(NPASS, npass)
    gcount = [0]

    def load_pass(p):
        t0 = p * TP
        ps2 = p % 2
        P.op("sp", lambda e: e.dma_start(out=hp[ps2], in_=hn2T_d[:, :, t0:t0 + TP]), writes=[t_hp[ps2]], dma=f"hp{ps2}")
        P.op("sp", lambda e: e.dma_start(out=rt[ps2], in_=rt_d[:, :, t0:t0 + TP]), writes=[t_rt[ps2]], dma=f"rtl{ps2}")

    def g_tokens(p, q, k0, k1):
        gi = p % NG
        ps2 = p % 2
        if k0 == 0:
            gcount[0] += 1
        gb_ = 6 + (gcount[0] % 2)
        for k in range(k0, k1):
            t = q * 4 + k
            sl = t % NPQ
            P.op("dve", lambda e, sl=sl, t=t: e.tensor_scalar(Pb[sl], iota_f, rt[ps2][:, 0, t:t + 1], rt[ps2][:, 2, t:t + 1], op0=ALU.is_equal, op1=ALU.mult),
                 reads=[t_rt[ps2], t_cpk], writes=[t_Pb[sl]])
            P.op("dve", lambda e, sl=sl, t=t: e.tensor_scalar(Qb[sl], iota_f, rt[ps2][:, 1, t:t + 1], None, op0=ALU.is_equal),
                 reads=[t_rt[ps2], t_cpk], writes=[t_Qb[sl]])
            P.op("pe", lambda e, k=k, sl=sl: e.matmul(bank(gb_)[:, k * 128:(k + 1) * 128], lhsT=Pb[sl], rhs=Qb[sl], start=True, stop=True),
                 reads=[t_Pb[sl], t_Qb[sl]], writes=[bank_t[gb_]])
        if k1 == 4:
            def ev():
                copy_op("act", G[gi][:, q * 4:(q + 1) * 4, :], bank(gb_).rearrange("p (t j) -> p t j", t=4), [bank_t[gb_]], [t_Gq[gi][q]])
            return ev
        return None

    def hidden(p, j):
        n = p * 128 + j
        us = n % NUV
        ps2 = p % 2
        P.op("sp", lambda e: e.dma_start(out=ub[us].rearrange("p c i -> p (c i)"), in_=ubf_d[j * 128:(j + 1) * 128, :]), writes=[t_ub[us]], dma=f"u{us}", extra=cv_deps)
        P.op("sp", lambda e: e.dma_start(out=vb[us], in_=vbf_d[j * 128:(j + 1) * 128, :]), writes=[t_vb[us]], dma=f"v{us}", extra=cv_deps)
        hbk = 4 + (n % 2)
        for c in range(8):
            P.op("pe", lambda e, c=c: e.matmul(bank(hbk)[:, 0:TP], lhsT=ub[us][:, c, :], rhs=hp[ps2][:, c, :], start=(c == 0), stop=(c == 7)),
                 reads=[t_ub[us], t_hp[ps2]], writes=[bank_t[hbk]])

    load_pass(0)
    for q in range(TP // 4):
        g_tokens(0, q, 0, 4)()
    for p in range(npass_run):
        gi = p % NG
        if p + 1 < npass_run:
            load_pass(p + 1)
        hidden(p, 0)
        for j in range(128):
            n = p * 128 + j
            us = n % NUV
            hbk = 4 + (n % 2)
            gs = n % NGE
            ev = None
            if p + 1 < npass_run:
                ev = g_tokens(p + 1, j // 2, (j % 2) * 2, (j % 2) * 2 + 2)
            if j + 1 < 128:
                hidden(p, j + 1)
            P.op("act", lambda e, hbk=hbk, gs=gs: e.activation(out=geb[gs], in_=bank(hbk)[:, 0:TP], func=AF.Gelu), reads=[bank_t[hbk]], writes=[t_geb[gs]])
            if ev is not None:
                ev()
            P.op("dve", lambda e, gs=gs, gi=gi, j=j: e.tensor_tensor(out=agb[gs], in0=geb[gs], in1=G[gi][:, :, j], op=ALU.mult),
                 reads=[t_geb[gs], t_G[gi]], writes=[t_agb[gs]])
            for ts in range(2):
                for dh in range(2):
                    ob = ts * 2 + dh
                    P.op("pe", lambda e, ob=ob, ts=ts, dh=dh, gs=gs, us=us, j=j: e.matmul(bank(ob), lhsT=agb[gs][:, ts * 128:(ts + 1) * 128], rhs=vb[us][:, dh * 512:(dh + 1) * 512], start=(j == 0), stop=(j == 127)),
                         reads=[t_agb[gs], t_vb[us]], writes=[bank_t[ob]])
        for ts in range(2):
            T = p * 2 + ts
            hs_ = T % 2
            P.op("sp", lambda e, T=T, hs_=hs_: e.dma_start(out=ht[hs_], in_=h_d[T * 128:(T + 1) * 128, :]), writes=[t_ht[hs_]], dma=f"ht{hs_}")
            P.op("dve", lambda e, hs_=hs_, ts=ts: e.tensor_tensor(out=ht[hs_].rearrange("p (a b) -> p a b", a=2), in0=ht[hs_].rearrange("p (a b) -> p a b", a=2), in1=psum[:, ts * 2:ts * 2 + 2, :], op=ALU.add),
                 reads=[bank_t[ts * 2], bank_t[ts * 2 + 1], t_ht[hs_]], writes=[t_ht[hs_]])
            rstd, t_r = rstd_from(ht[hs_], [t_ht[hs_]], 1024)
            P.op("dve", lambda e, hs_=hs_, rstd=rstd: e.scalar_tensor_tensor(out=ht[hs_], in0=ht[hs_], scalar=rstd, in1=gfin, op0=ALU.mult, op1=ALU.mult),
                 reads=[t_ht[hs_], t_r, t_gfin], writes=[t_ht[hs_]])
            finals.append(P.op("sp", lambda e, T=T, hs_=hs_: e.dma_start(out=out_d[T * 128:(T + 1) * 128, :], in_=ht[hs_]), reads=[t_ht[hs_]], dma=f"out{hs_}"))

    P.emit(final_waits=finals[-2:])
    st.close()
    return nc


def _t5_bucket(n):
    n = np.maximum(n, 0)
    max_exact = 16
    nf = np.maximum(n, max_exact).astype(np.float32)
    large = max_exact + (np.log(nf / np.float32(max_exact)) / np.float32(math.log(128 / max_exact)) * np.float32(32 - max_exact)).astype(np.int32)
    large = np.minimum(large, 31)
    return np.where(n < max_exact, n, large)


def prepare_inputs(inp):
    f = lambda a: np.ascontiguousarray(np.asarray(a, dtype=np.float32))
    x = f(inp["x"])
    w_in = f(inp["w_in"][0]); w_out = f(inp["w_out"][0]); w_q = f(inp["peer_w_q"][0])
    sk = f(inp["peer_sub_keys"][0]).reshape(16, 128, 128)
    skT = np.ascontiguousarray(sk.transpose(2, 0, 1)).reshape(128, 2048)
    u = f(inp["peer_u"][0]); v = f(inp["peer_v"][0])
    uT = np.ascontiguousarray(u.reshape(128, 128, 8, 128).transpose(1, 3, 2, 0)).reshape(16384, 1024)
    vP = np.ascontiguousarray(v.reshape(128, 128, 1024).transpose(1, 0, 2)).reshape(16384, 1024)
    table = f(inp["rel_bias_table"])
    pp = np.arange(128)[:, None]; jj = np.arange(1024)[None, :]
    n = jj - pp - 384
    bidx = _t5_bucket(n)
    wbias = np.empty((128, 4, 1024), np.float32)
    for h in range(4):
        wbias[:, h, :] = np.where(n >= 0, table[bidx, h], np.float32(-30000.0))
    cpk = np.zeros((128, 848), np.float32)
    cpk[:, 0:128] = np.arange(128, dtype=np.float32)[None, :]
    cpk[:, 128:256] = np.eye(128, dtype=np.float32)
    cpk[:, 256:320] = (np.arange(64)[None, :] >= (np.arange(128)[:, None] % 64)).astype(np.float32)
    cpk[:, 320:832] = (np.arange(512)[None, :] % 64 != 0).astype(np.float32)
    cpk[:, 832:848] = np.arange(16, dtype=np.float32)[None, :]
    ppk = np.zeros((128, 540), np.float32)
    ppk[:, 0:4] = table[31][None, :]
    lbl = f(inp["hgrn_lb_logits"]).reshape(2, 4, 128).transpose(2, 0, 1).reshape(128, 8)
    ppk[:, 4:12] = lbl
    lamv = np.stack([f(inp["diff_lambda_q1"][0]), f(inp["diff_lambda_k1"][0]), f(inp["diff_lambda_q2"][0]), f(inp["diff_lambda_k2"][0])]).reshape(1, 256)
    ppk[:, 12:268] = lamv
    ppk[:, 268:276] = f(inp["norm1_g"][0]).reshape(8, 128).T
    ppk[:, 276:284] = f(inp["norm2_g"][0]).reshape(8, 128).T
    ppk[:, 284:412] = f(inp["diff_subln_g"][0])[None, :]
    ppk[:, 412:540] = f(inp["hgrn_gnorm_g"][0])[None, :]
    gfin = np.ascontiguousarray(np.broadcast_to(f(inp["final_norm_g"])[None, :], (128, 1024)))
    shared = dict(w_in=w_in, w_out=w_out, w_q=w_q, skT=skT, uT=uT, vP=vP, wbias=wbias, cpk=cpk, ppk=ppk, gfin=gfin)
    xs = x.reshape(NCORES, NTOK, 1024)
    return [dict(shared, x=np.ascontiguousarray(xs[c])) for c in range(NCORES)]


_NC_CACHE = {}


def kernel(**inputs):
    in_maps = prepare_inputs(inputs)
    if "nc" not in _NC_CACHE:
        _NC_CACHE["nc"] = build(False)
    res = run_bass_kernel_spmd(_NC_CACHE["nc"], in_maps, core_ids=list(range(NCORES)))
    out = np.stack([np.asarray(r["out"]) for r in res.results]).reshape(16, 2048, 1024)
    return out.astype(np.float32)
```
